# Optimizing a Trainium2 kernel written in Bass

```python
import math
import jax, jax.numpy as jnp
from jax import lax
import numpy as np

D_MODEL = 1024
BATCH = 1
SEQ = 16384
DEPTH = 1

D_MIX = D_MODEL
MLA_HEADS = 8
MLA_NOPE = 64
MLA_ROPE = 32
MLA_V = 64
MLA_Q_RANK = 384
MLA_KV_RANK = 256
ROPE_BASE = 10000.0
MOBA_HEADS = 8
MOBA_HEAD_DIM = 64
MOBA_BLOCK = 256
MOBA_TOPK = 3
Q_BLOCK = 128
D_FF = 2816
FFN_RES = 0.5
EPS = 1e-6
NEG_INF = -1e30

IN_SPLITS = (MLA_Q_RANK, MLA_KV_RANK, MLA_ROPE,
             MOBA_HEADS * MOBA_HEAD_DIM, MOBA_HEADS * MOBA_HEAD_DIM, MOBA_HEADS * MOBA_HEAD_DIM)
D_IN = sum(IN_SPLITS)
D_CAT = MLA_HEADS * MLA_V + MOBA_HEADS * MOBA_HEAD_DIM

kernel_name = "hybrid_mla_moba_macaron"


def rms_norm(x, g):
    xf = x.astype(jnp.float32)
    y = xf * lax.rsqrt(jnp.mean(xf * xf, axis=-1, keepdims=True) + EPS)
    return (y * g.astype(jnp.float32)).astype(x.dtype)


def swiglu(x, w_gate, w_up, w_down):
    return (jax.nn.silu(x @ w_gate) * (x @ w_up)) @ w_down


def apply_rope(x, pos):
    r = x.shape[-1]
    half = r // 2
    inv = jnp.power(ROPE_BASE, -jnp.arange(half, dtype=jnp.float32) * 2.0 / r)
    ang = pos.astype(jnp.float32)[:, :, None, None] * inv
    cos, sin = jnp.cos(ang), jnp.sin(ang)
    xf = x.astype(jnp.float32)
    x1, x2 = xf[..., :half], xf[..., half:]
    out = jnp.concatenate([x1 * cos - x2 * sin, x1 * sin + x2 * cos], axis=-1)
    return out.astype(x.dtype)


def alibi_slopes(n_heads):
    return jnp.exp2(-8.0 * jnp.arange(1, n_heads + 1, dtype=jnp.float32) / n_heads)


def mla_attention(c_q, c_kv, k_rope, pos, q_norm, w_uq, kv_norm, w_ukv):
    B, S, _ = c_q.shape
    H = MLA_HEADS
    q = (rms_norm(c_q, q_norm) @ w_uq).reshape(B, S, H, MLA_NOPE + MLA_ROPE)
    q = jnp.concatenate([q[..., :MLA_NOPE], apply_rope(q[..., MLA_NOPE:], pos)], axis=-1)
    kv = (rms_norm(c_kv, kv_norm) @ w_ukv).reshape(B, S, H, MLA_NOPE + MLA_V)
    k_nope, v = kv[..., :MLA_NOPE], kv[..., MLA_NOPE:]
    k_r = apply_rope(k_rope[:, :, None, :], pos)
    k = jnp.concatenate([k_nope, jnp.broadcast_to(k_r, (B, S, H, MLA_ROPE))], axis=-1)
    scale = (MLA_NOPE + MLA_ROPE) ** -0.5
    key_idx = jnp.arange(S)

    def one_block(start):
        qb = lax.dynamic_slice_in_dim(q, start, Q_BLOCK, axis=1)
        s = jnp.einsum('bqhd,bkhd->bhqk', qb, k).astype(jnp.float32) * scale
        q_idx = start + jnp.arange(Q_BLOCK)
        s = jnp.where(key_idx[None, :] <= q_idx[:, None], s, NEG_INF)
        p = jax.nn.softmax(s, axis=-1).astype(v.dtype)
        return jnp.einsum('bhqk,bkhd->bqhd', p, v)

    out = lax.map(one_block, jnp.arange(0, S, Q_BLOCK))
    return jnp.moveaxis(out, 0, 1).reshape(B, S, H * MLA_V)


def moba_attention(q, k, v, pos):
    B, S, H, D = q.shape
    L = MOBA_BLOCK
    NB = -(-S // L)
    pad = NB * L - S
    kp = jnp.pad(k, ((0, 0), (0, pad), (0, 0), (0, 0)))
    vp = jnp.pad(v, ((0, 0), (0, pad), (0, 0), (0, 0)))
    pp = jnp.pad(pos, ((0, 0), (0, pad)))
    k_sel = min(MOBA_TOPK, NB)
    scale = D ** -0.5
    slopes = alibi_slopes(H)
    kb = kp.reshape(B, NB, L, H, D)
    k_mean = jnp.mean(kb.astype(jnp.float32), axis=2).astype(k.dtype)
    kb_h = kb.transpose(0, 3, 1, 2, 4)
    vb_h = vp.reshape(B, NB, L, H, D).transpose(0, 3, 1, 2, 4)
    pos_b = pp.reshape(B, NB, L)
    bi = jnp.arange(B)[:, None, None, None]
    hi = jnp.arange(H)[None, :, None, None]

    def one_block(start):
        qb = lax.dynamic_slice_in_dim(q, start, Q_BLOCK, axis=1)
        pq = lax.dynamic_slice_in_dim(pos, start, Q_BLOCK, axis=1)
        own = start // L
        gate = jnp.einsum('bqhd,bnhd->bhqn', qb, k_mean).astype(jnp.float32)
        gate = jnp.where((jnp.arange(NB) < own)[None, None, None, :], gate, NEG_INF)
        _, idx = lax.top_k(gate, k_sel)
        valid = jnp.arange(k_sel) < own
        kg = kb_h[bi, hi, idx]
        vg = vb_h[bi, hi, idx]
        pg = pos_b[bi, idx]
        dist_sel = jnp.abs(pq[:, None, :, None, None] - pg).astype(jnp.float32)
        s_sel = jnp.einsum('bqhd,bhqkld->bhqkl', qb, kg).astype(jnp.float32) * scale
        s_sel = s_sel - slopes[None, :, None, None, None] * dist_sel
        s_sel = jnp.where(valid[None, None, None, :, None], s_sel, NEG_INF)
        k_own = lax.dynamic_slice_in_dim(kp, own * L, L, axis=1)
        v_own = lax.dynamic_slice_in_dim(vp, own * L, L, axis=1)
        p_own_pos = lax.dynamic_slice_in_dim(pp, own * L, L, axis=1)
        dist_own = jnp.abs(pq[:, None, :, None] - p_own_pos[:, None, None, :]).astype(jnp.float32)
        s_own = jnp.einsum('bqhd,blhd->bhql', qb, k_own).astype(jnp.float32) * scale
        s_own = s_own - slopes[None, :, None, None] * dist_own
        causal = (own * L + jnp.arange(L))[None, :] <= (start + jnp.arange(Q_BLOCK))[:, None]
        s_own = jnp.where(causal[None, None], s_own, NEG_INF)
        s = jnp.concatenate([s_sel.reshape(B, H, Q_BLOCK, k_sel * L), s_own], axis=-1)
        p = jax.nn.softmax(s, axis=-1).astype(v.dtype)
        p_sel = p[..., :k_sel * L].reshape(B, H, Q_BLOCK, k_sel, L)
        p_o = p[..., k_sel * L:]
        return (jnp.einsum('bhqkl,bhqkld->bqhd', p_sel, vg)
                + jnp.einsum('bhql,blhd->bqhd', p_o, v_own))

    out = lax.map(one_block, jnp.arange(0, S, Q_BLOCK))
    return jnp.moveaxis(out, 0, 1).reshape(B, S, H * D)


def setup_inputs(seed: int = 0) -> dict:
    key = jax.random.key(seed)
    ks = jax.random.split(key, 20)

    def dense(k, fan_in, fan_out):
        return jax.random.normal(k, (DEPTH, fan_in, fan_out), jnp.float32) * fan_in ** -0.5

    def gain(k, n):
        return 1.0 + 0.02 * jax.random.normal(k, (DEPTH, n), jnp.float32)

    x = jax.random.normal(ks[0], (BATCH, SEQ, D_MODEL), jnp.float32)
    positions = jnp.broadcast_to(jnp.arange(SEQ, dtype=jnp.int32)[None, :], (BATCH, SEQ))
    return {
        "x": x,
        "positions": positions,
        "ffn1_norm": gain(ks[1], D_MODEL),
        "ffn1_w_gate": dense(ks[2], D_MODEL, D_FF),
        "ffn1_w_up": dense(ks[3], D_MODEL, D_FF),
        "ffn1_w_down": dense(ks[4], D_FF, D_MODEL),
        "mix_norm": gain(ks[5], D_MODEL),
        "w_in": dense(ks[6], D_MODEL, D_IN),
        "mla_q_norm": gain(ks[7], MLA_Q_RANK),
        "mla_w_uq": dense(ks[8], MLA_Q_RANK, MLA_HEADS * (MLA_NOPE + MLA_ROPE)),
        "mla_kv_norm": gain(ks[9], MLA_KV_RANK),
        "mla_w_ukv": dense(ks[10], MLA_KV_RANK, MLA_HEADS * (MLA_NOPE + MLA_V)),
        "w_out": dense(ks[11], D_CAT, D_MODEL),
        "ffn2_norm": gain(ks[12], D_MODEL),
        "ffn2_w_gate": dense(ks[13], D_MODEL, D_FF),
        "ffn2_w_up": dense(ks[14], D_MODEL, D_FF),
        "ffn2_w_down": dense(ks[15], D_FF, D_MODEL),
        "final_norm": 1.0 + 0.02 * jax.random.normal(ks[16], (D_MODEL,), jnp.float32),
    }


def reference(x, positions, ffn1_norm, ffn1_w_gate, ffn1_w_up, ffn1_w_down, mix_norm, w_in,
              mla_q_norm, mla_w_uq, mla_kv_norm, mla_w_ukv, w_out, ffn2_norm, ffn2_w_gate,
              ffn2_w_up, ffn2_w_down, final_norm):
    B, S, _ = x.shape
    cuts = np.cumsum(IN_SPLITS)[:-1].tolist()
    for l in range(DEPTH):
        x = x + FFN_RES * swiglu(rms_norm(x, ffn1_norm[l]), ffn1_w_gate[l], ffn1_w_up[l], ffn1_w_down[l])
        u = rms_norm(x, mix_norm[l]) @ w_in[l]
        c_q, c_kv, k_rope, mq, mk, mv = jnp.split(u, cuts, axis=-1)
        a_out = mla_attention(c_q, c_kv, k_rope, positions, mla_q_norm[l], mla_w_uq[l],
                              mla_kv_norm[l], mla_w_ukv[l])
        shp = (B, S, MOBA_HEADS, MOBA_HEAD_DIM)
        b_out = moba_attention(mq.reshape(shp), mk.reshape(shp), mv.reshape(shp), positions)
        x = x + jnp.concatenate([a_out, b_out], axis=-1) @ w_out[l]
        x = x + FFN_RES * swiglu(rms_norm(x, ffn2_norm[l]), ffn2_w_gate[l], ffn2_w_up[l], ffn2_w_down[l])
    return rms_norm(x, final_norm)
```

```python
import contextlib
import math
import numpy as np
import ml_dtypes
import concourse.bass as bass
import concourse.mybir as mybir
from concourse.bass_utils import run_bass_kernel_spmd

F32 = mybir.dt.float32
BF16 = mybir.dt.bfloat16
I32 = mybir.dt.int32
AF = mybir.ActivationFunctionType
ALU = mybir.AluOpType
AX = mybir.AxisListType

NCORES = 8
S = 16384
D = 1024
DFF = 2816
NF = DFF // 128
TC = S // NCORES
EPS = 1e-6


class Buf:
    __slots__ = ("name", "w", "r", "sem", "dcount", "epoch")

    def __init__(self, name):
        self.name = name
        self.w = {}
        self.r = {}
        self.sem = None
        self.dcount = 0
        self.epoch = None


class Sched:
    ENGS = ("pe", "act", "dve", "pool", "sp")

    def __init__(self, nc, stack):
        self.nc = nc
        self.stack = stack
        self.streams = {e: [] for e in self.ENGS}
        self.sem = {e: stack.enter_context(nc.semaphore("s_" + e)) for e in self.ENGS}
        self.cnt = {e: 0 for e in self.ENGS}
        self.seen = {e: {} for e in self.ENGS}
        self.nsem = len(self.ENGS)
        self.dtoks = {}
        self.epoch_n = 0

    def _need(self, eng, waits, tok):
        if tok is None:
            return
        sem, val = tok
        if eng == "pe" and sem is self.sem["pe"]:
            return
        k = id(sem)
        if self.seen[eng].get(k, 0) >= val:
            return
        if k not in waits or waits[k][1] < val:
            waits[k] = (sem, val)

    def _deps(self, eng, reads, writes, epoch=None):
        waits = {}
        for b in reads:
            for t in b.w.values():
                self._need(eng, waits, t)
        for b in writes:
            if not (epoch is not None and b.epoch == epoch):
                for t in b.w.values():
                    self._need(eng, waits, t)
            for t in b.r.values():
                self._need(eng, waits, t)
        for k, (sem, val) in waits.items():
            self.seen[eng][k] = val
        return list(waits.values())

    def _commit(self, tok, reads, writes, epoch=None):
        for b in reads:
            b.r[id(tok[0])] = tok
        for b in writes:
            if epoch is not None and b.epoch == epoch:
                b.w[id(tok[0])] = tok
            else:
                b.w = {id(tok[0]): tok}
            b.r = {}
            b.epoch = epoch

    def op(self, eng, fn, reads=(), writes=()):
        waits = self._deps(eng, reads, writes)
        self.cnt[eng] += 1
        tok = (self.sem[eng], self.cnt[eng])
        self.streams[eng].append((waits, fn, tok[0], 1))
        self._commit(tok, reads, writes)
        return tok

    def dma(self, eng, out, in_, reads=(), writes=(), epoch=None, carrier=None):
        waits = self._deps(eng, reads, writes, epoch)
        b = carrier if carrier is not None else writes[0]
        if b.sem is None:
            b.sem = self.stack.enter_context(self.nc.semaphore(f"d{self.nsem}_" + b.name))
            self.nsem += 1
        b.dcount += 16
        tok = (b.sem, b.dcount)
        self.dtoks[id(b.sem)] = tok
        self.streams[eng].append((waits, lambda e: e.dma_start(out=out, in_=in_), tok[0], 16))
        self._commit(tok, reads, writes, epoch)
        return tok

    def raw(self, eng, fn, sem, inc, reads=(), writes=()):
        waits = self._deps(eng, reads, writes)
        b = writes[0]
        if b.sem is None:
            b.sem = sem
        b.dcount += inc
        tok = (b.sem, b.dcount)
        self.dtoks[id(b.sem)] = tok
        self.streams[eng].append((waits, fn, tok[0], inc))
        self._commit(tok, reads, writes)
        return tok

    def dma_fn(self, eng, fn, reads=(), writes=(), epoch=None, carrier=None):
        waits = self._deps(eng, reads, writes, epoch)
        b = carrier if carrier is not None else writes[0]
        if b.sem is None:
            b.sem = self.stack.enter_context(self.nc.semaphore(f"d{self.nsem}_" + b.name))
            self.nsem += 1
        b.dcount += 16
        tok = (b.sem, b.dcount)
        self.dtoks[id(b.sem)] = tok
        self.streams[eng].append((waits, fn, tok[0], 16))
        self._commit(tok, reads, writes, epoch)
        return tok

    def barrier(self):
        toks = [(self.sem[e], self.cnt[e]) for e in self.ENGS if self.cnt[e] > 0] + list(self.dtoks.values())
        for en in self.ENGS:
            waits = {}
            for t in toks:
                self._need(en, waits, t)
            for k, (sem, val) in waits.items():
                self.seen[en][k] = val
            self.streams[en].append((list(waits.values()), None, None, 0))
        self.epoch_n += 1
        for e in self.ENGS:
            self.sem[e] = self.stack.enter_context(self.nc.semaphore(f"s{self.epoch_n}_" + e))
            self.cnt[e] = 0
            self.nsem += 1

    def final_wait(self, eng, bufs):
        waits = self._deps(eng, bufs, ())
        self.streams[eng].append((waits, None, None, 0))

    def emit(self, block):
        nc = self.nc
        hmap = {"pe": block.tensor, "act": block.scalar, "dve": block.vector, "pool": block.gpsimd,
                "sp": block.sync}
        for en in self.ENGS:
            stream = self.streams[en]

            def body(e, stream=stream):
                for waits, fn, sem, inc in stream:
                    for (ws, wv) in waits:
                        e.wait_ge(ws, wv)
                    if fn is not None:
                        ins = fn(e)
                        ins.then_inc(sem, inc)

            hmap[en](body)


class Ctx:
    def __init__(self, nc, stack, arena_elems):
        self.nc = nc
        self.stack = stack
        self.arena = stack.enter_context(nc.sbuf_tensor("arena", [128, arena_elems], BF16))
        self.N = arena_elems
        self.off = 0
        self.PS = []
        for i in range(8):
            t = stack.enter_context(nc.psum_tensor(f"psb{i}", [128, 512], F32))
            self.PS.append((t[:, :], Buf(f"ps{i}")))

    def sb(self, name, shape, dt):
        n = 1
        for d_ in shape[1:]:
            n *= d_
        ne = n * (1 if dt == BF16 else 2)
        off = (self.off + 15) // 16 * 16
        assert off + ne <= self.N, f"arena overflow at {name}: {off + ne} > {self.N}"
        v = self.arena[:, off:off + ne]
        if dt != BF16:
            v = v.bitcast(dt)
        if len(shape) == 3:
            v = v.rearrange("p (a b) -> p a b", a=shape[1])
        self.off = off + ne
        return v

    def mark(self):
        return self.off

    def release(self, m):
        self.off = m


BIG = 30000.0
NG = S // 512
NGR = NG
MSUB = 9
STAGE = 9
PI = math.pi


def alloc_common(sc, cx, dr):
    R = {}
    R["ones"] = cx.sb("ones", [128, 128], BF16)
    R["epsc"] = cx.sb("epsc", [128, 1], F32)
    R["gv"] = cx.sb("gv", [128, 37], F32)
    R["cb"] = Buf("consts")
    sc.op("dve", lambda e: e.memset(R["ones"][:, :], 1.0), writes=[R["cb"]])
    sc.op("dve", lambda e: e.memset(R["epsc"][:, :], EPS), writes=[R["cb"]])
    R["gvb"] = Buf("gv")
    sc.dma("sp", R["gv"][:, :], dr["gvec"], writes=[R["gvb"]])
    R["sq"] = [(cx.sb(f"sq{i}", [128, 512], BF16), Buf(f"sq{i}")) for i in range(2)]
    R["std"] = (cx.sb("std", [128, 512], F32), Buf("std"))
    R["rstd"] = (cx.sb("rstd", [128, 512], F32), Buf("rstd"))
    R["ss"] = cx.PS[0]
    return R


def rmsnorm(sc, R, nch, n, xin, xbufs, gcol, out, obuf, dim):
    ss = R["ss"]
    for c in range(nch):
        sq = R["sq"][c % 2]
        sc.op("act", lambda e, c=c, sq=sq: e.activation(out=sq[0][:, 0:n], in_=xin(c), func=AF.Square),
              reads=[xbufs[c]], writes=[sq[1]])
        sc.op("pe", lambda e, c=c, sq=sq: e.matmul(ss[0][:, 0:n], lhsT=R["ones"][:, :], rhs=sq[0][:, 0:n],
                                                  start=(c == 0), stop=(c == nch - 1)),
              reads=[sq[1], R["cb"]], writes=[ss[1]])
    sd, rs = R["std"], R["rstd"]
    sc.op("act", lambda e: e.activation(out=sd[0][:, 0:n], in_=ss[0][:, 0:n], func=AF.Sqrt,
                                        bias=R["epsc"][:, 0:1], scale=1.0 / dim),
          reads=[ss[1], R["cb"]], writes=[sd[1]])
    sc.op("dve", lambda e: e.reciprocal(out=rs[0][:, 0:n], in_=sd[0][:, 0:n]), reads=[sd[1]], writes=[rs[1]])
    for c in range(nch):
        sc.op("dve", lambda e, c=c: e.scalar_tensor_tensor(out=out(c), in0=xin(c), scalar=gcol[:, c:c + 1],
                                                          in1=rs[0][:, 0:n], op0=ALU.mult, op1=ALU.mult),
              reads=[xbufs[c], rs[1], R["gvb"]], writes=[obuf])


def alloc_ffn(sc, cx):
    F = {}
    F["hT"] = (cx.sb("hT", [128, 8, 1024], BF16), [Buf("hT0"), Buf("hT1")])
    F["actT"] = (cx.sb("actT", [128, NF, 1024], BF16), [[Buf(f"act{f}_{j}") for j in range(2)] for f in range(NF)])
    F["wg"] = [(cx.sb(f"wg{i}", [128, 8, 128], BF16), Buf(f"wg{i}")) for i in range(3)]
    F["wu"] = [(cx.sb(f"wu{i}", [128, 8, 128], BF16), Buf(f"wu{i}")) for i in range(3)]
    F["wd"] = [(cx.sb(f"wd{i}", [128, NF, 128], BF16), Buf(f"wd{i}")) for i in range(2)]
    F["sg"] = [(cx.sb(f"sg{i}", [128, 512], F32), Buf(f"sg{i}")) for i in range(2)]
    F["g_ps"] = [cx.PS[1], cx.PS[2]]
    F["u_ps"] = [cx.PS[3], cx.PS[4]]
    F["o_ps"] = [cx.PS[5], cx.PS[6]]
    return F


def emit_ffn(sc, R, F, xT, xb, wg_d, wu_d, wd_d, gcol):
    hT, hb = F["hT"]
    actT, ab = F["actT"]
    for half in range(2):
        tgs = [half * 2, half * 2 + 1]
        for j, tg in enumerate(tgs):
            rmsnorm(sc, R, 8, 512, lambda c, tg=tg: xT[:, c, tg * 512:(tg + 1) * 512], [xb[c][tg] for c in range(8)],
                    gcol, lambda c, j=j: hT[:, c, j * 512:(j + 1) * 512], hb[j], D)
        for f in range(NF):
            wgs = F["wg"][f % 3]
            wus = F["wu"][f % 3]
            sc.dma("pool", wgs[0][:, :, :], wg_d[f], writes=[wgs[1]])
            sc.dma("pool", wus[0][:, :, :], wu_d[f], writes=[wus[1]])
            for j in range(2):
                gp = F["g_ps"][(f * 2 + j) % 2]
                up = F["u_ps"][(f * 2 + j) % 2]
                for k in range(8):
                    sc.op("pe", lambda e, k=k, gp=gp, wgs=wgs, j=j: e.matmul(
                        gp[0][:, :], lhsT=wgs[0][:, k, :], rhs=hT[:, k, j * 512:(j + 1) * 512],
                        start=(k == 0), stop=(k == 7)), reads=[wgs[1], hb[j]], writes=[gp[1]])
                for k in range(8):
                    sc.op("pe", lambda e, k=k, up=up, wus=wus, j=j: e.matmul(
                        up[0][:, :], lhsT=wus[0][:, k, :], rhs=hT[:, k, j * 512:(j + 1) * 512],
                        start=(k == 0), stop=(k == 7)), reads=[wus[1], hb[j]], writes=[up[1]])
                sg = F["sg"][(f * 2 + j) % 2]
                sc.op("act", lambda e, gp=gp, sg=sg: e.activation(out=sg[0][:, :], in_=gp[0][:, :], func=AF.Silu),
                      reads=[gp[1]], writes=[sg[1]])
                sc.op("dve", lambda e, sg=sg, up=up, f=f, j=j: e.tensor_tensor(
                    out=actT[:, f, j * 512:(j + 1) * 512], in0=sg[0][:, :], in1=up[0][:, :], op=ALU.mult),
                      reads=[sg[1], up[1]], writes=[ab[f][j]])
        for d in range(8):
            wds = F["wd"][d % 2]
            sc.dma("pool", wds[0][:, :, :], wd_d[d], writes=[wds[1]])
            for j, tg in enumerate(tgs):
                op_ = F["o_ps"][(d * 2 + j) % 2]
                for f in range(NF):
                    sc.op("pe", lambda e, f=f, op_=op_, wds=wds, j=j: e.matmul(
                        op_[0][:, :], lhsT=wds[0][:, f, :], rhs=actT[:, f, j * 512:(j + 1) * 512],
                        start=(f == 0), stop=(f == NF - 1)), reads=[wds[1], ab[f][j]], writes=[op_[1]])
                sc.op("dve", lambda e, op_=op_, d=d, tg=tg: e.scalar_tensor_tensor(
                    out=xT[:, d, tg * 512:(tg + 1) * 512], in0=op_[0][:, :], scalar=0.5,
                    in1=xT[:, d, tg * 512:(tg + 1) * 512], op0=ALU.mult, op1=ALU.add),
                      reads=[op_[1], xb[d][tg]], writes=[xb[d][tg]])


def load_w(sc, cx, dr, name, shape):
    t = cx.sb(name, shape, BF16)
    b = Buf(name)
    sc.dma("pool", t, dr[name], writes=[b])
    return (t, b)


def emit_attention(sc, cx, R, dr, h2_all, cat_loc, catb):
    PS = cx.PS
    gv = R["gv"]
    cv = cx.sb("cvec", [128, 8], F32)
    cvb = Buf("cvec")
    sc.dma("sp", cv[:, :], dr["cvec"], writes=[cvb])
    pic = cx.sb("pic", [128, 1], F32)
    sc.op("dve", lambda e: e.memset(pic[:, :], 0.0), writes=[cvb])
    ident = load_w(sc, cx, dr, "ident", [128, 128])
    maskc = load_w(sc, cx, dr, "maskc", [128, 4, 512])
    blkidx = cx.sb("blkidx", [128, 64], F32)
    blkb = Buf("blkidx")
    sc.dma("sp", blkidx[:, :], dr["blkidx"], writes=[blkb])
    Q1 = cx.sb("Q1", [128, S], BF16)
    K1 = cx.sb("K1", [128, S], BF16)
    QB = cx.sb("QB", [128, S // 2], BF16)
    V = cx.sb("V", [128, 128, 96], BF16)
    qb = [Buf(f"q{g}") for g in range(NG)]
    kb = [Buf(f"k{g}") for g in range(NG)]
    vb = [Buf(f"v{g}") for g in range(NG)]
    qbb = [Buf(f"qB{g}") for g in range(NG)]
    vones = Buf("vones")
    sc.op("pool", lambda e: e.memset(V[:, :, 64:96], 1.0), writes=[vones])
    h2g = [(cx.sb(f"h2g{i}", [128, 8, 512], BF16), Buf(f"h2g{i}")) for i in range(2)]
    posi = (cx.sb("posi", [128, 512], I32), Buf("posi"))
    posf = (cx.sb("posf", [128, 512], F32), Buf("posf"))
    tA = (cx.sb("tA", [128, 512], F32), Buf("tA"))
    tB = (cx.sb("tB", [128, 512], F32), Buf("tB"))
    tI = (cx.sb("tI", [128, 512], I32), Buf("tI"))
    cosT = (cx.sb("cosT", [128, 512], F32), Buf("cosT"))
    sinT = (cx.sb("sinT", [128, 512], F32), Buf("sinT"))
    tm1 = (cx.sb("tm1", [128, 512], F32), Buf("tm1"))
    tm2 = (cx.sb("tm2", [128, 512], F32), Buf("tm2"))
    Pb = [(cx.sb(f"P{i}", [128, 512], BF16), Buf(f"P{i}")) for i in range(3)]
    rec = (cx.sb("rec", [128, 512], F32), Buf("rec"))
    aT = [(cx.sb(f"aT{i}", [128, 512], BF16), Buf(f"aT{i}")) for i in range(2)]
    h2v = h2_all.rearrange("(r c p) t -> r p c t", c=8, p=128)

    def load_h2(g):
        t = h2g[g % 2]
        sc.dma("sp" if h2_all.dtype == BF16 else "pool", t[0][:, :, :], h2v[g // 4][:, :, (g % 4) * 512:(g % 4 + 1) * 512],
               reads=[R["h2all_b"]], writes=[t[1]])
        return t

    def load_pos(g):
        sc.dma("sp", posi[0][:, :], dr["pos"][g * 512:(g + 1) * 512].partition_broadcast(128), writes=[posi[1]])
        sc.op("dve", lambda e: e.tensor_copy(out=posf[0][:, :], in_=posi[0][:, :]), reads=[posi[1]], writes=[posf[1]])

    def attention_pass(Qfn, Kt, nrows, scale, row0, tagbase):
        def group(qg, blk):
            nk = 4 * qg + 4
            Ob = PS[3 + (qg % 2)]
            q0 = qg * 512

            def qk(kt, blk):
                Sb = PS[blk % 3]
                Qt, qoff = Qfn(kt)
                diag = kt >= 4 * qg
                sc.op("pe", lambda e, Sb=Sb, kt=kt, Qt=Qt, qoff=qoff, diag=diag: e.matmul(
                    Sb[0][:, :], lhsT=Kt[0:nrows, kt * 128:(kt + 1) * 128], rhs=Qt[0:nrows, q0 - qoff:q0 - qoff + 512],
                    start=True, stop=not diag), reads=[kb[kt // 4], qb[qg], qbb[qg]], writes=[Sb[1]])
                if diag:
                    i = kt - 4 * qg
                    sc.op("pe", lambda e, Sb=Sb, i=i: e.matmul(Sb[0][:, :], lhsT=ident[0][:, :], rhs=maskc[0][:, i, :],
                                                               start=False, stop=True),
                          reads=[ident[1], maskc[1]], writes=[Sb[1]])

            qk(0, blk)
            for kt in range(nk):
                if kt + 1 < nk:
                    qk(kt + 1, blk + 1)
                Sb = PS[blk % 3]
                P = Pb[blk % 3]
                sc.op("act", lambda e, Sb=Sb, P=P: e.activation(out=P[0][:, :], in_=Sb[0][:, :], func=AF.Exp, scale=scale),
                      reads=[Sb[1]], writes=[P[1]])
                sc.op("pe", lambda e, P=P, kt=kt, Ob=Ob, nk=nk: e.matmul(
                    Ob[0][0:96, :], lhsT=V[:, kt, 0:96], rhs=P[0][:, :], start=(kt == 0), stop=(kt == nk - 1)),
                      reads=[P[1], vb[kt // 4], vones], writes=[Ob[1]])
                blk += 1
            sc.op("dve", lambda e, Ob=Ob: e.reciprocal(out=rec[0][0:32, :], in_=Ob[0][64:96, :]), reads=[Ob[1]], writes=[rec[1]])
            sc.op("dve", lambda e, Ob=Ob: e.reciprocal(out=rec[0][32:64, :], in_=Ob[0][64:96, :]), reads=[Ob[1]], writes=[rec[1]])
            at = aT[qg % 2]
            sc.op("dve", lambda e, at=at, Ob=Ob: e.tensor_tensor(out=at[0][0:64, :], in0=Ob[0][0:64, :], in1=rec[0][0:64, :], op=ALU.mult),
                  reads=[Ob[1], rec[1]], writes=[at[1]])
            sc.dma("sp", cat_loc[row0:row0 + 64, q0:q0 + 512], at[0][0:64, :], reads=[at[1]], writes=[catb], epoch="cat",
                   carrier=at[1])
            return blk

        blk = 0
        for qg in range(NGR):
            blk = group(qg, blk)

    m0 = cx.mark()
    wcq = load_w(sc, cx, dr, "w_cq", [128, 8, 384])
    wckv = load_w(sc, cx, dr, "w_ckv", [128, 8, 256])
    wkr = load_w(sc, cx, dr, "w_kr", [128, 8, 96])
    wkrs = load_w(sc, cx, dr, "w_krs", [128, 8, 96])
    wuq = load_w(sc, cx, dr, "w_uq", [128, 3, 96])
    wuqs = load_w(sc, cx, dr, "w_uqs", [128, 3, 96])
    wuk = load_w(sc, cx, dr, "w_uk", [128, 2, 64])
    wuv = load_w(sc, cx, dr, "w_uv", [128, 2, 64])
    cqf = (cx.sb("cqf", [128, 3, 512], F32), [Buf(f"cqf{c}") for c in range(3)])
    ckvf = (cx.sb("ckvf", [128, 2, 512], F32), [Buf(f"ckvf{c}") for c in range(2)])
    cqn = (cx.sb("cqn", [128, 3, 512], BF16), Buf("cqn"))
    ckvn = (cx.sb("ckvn", [128, 2, 512], BF16), Buf("ckvn"))
    rp = slice(64, 96)
    def mla_prep(g):
        hg = load_h2(g)
        load_pos(g)
        c0 = g * 512
        sc.op("dve", lambda e: e.tensor_scalar(out=tA[0][rp, :], in0=posf[0][rp, :], scalar1=cv[rp, 0:1], scalar2=None, op0=ALU.mult),
              reads=[posf[1], cvb], writes=[tA[1]])
        for which, tab in ((0, sinT), (1, cosT)):
            sc.op("dve", lambda e, which=which: e.tensor_scalar(out=tB[0][rp, :], in0=tA[0][rp, :], scalar1=1.0 / (2 * PI),
                                                                scalar2=0.25 * which, op0=ALU.mult, op1=ALU.add),
                  reads=[tA[1]], writes=[tB[1]])
            sc.op("dve", lambda e: e.tensor_copy(out=tI[0][rp, :], in_=tB[0][rp, :]), reads=[tB[1]], writes=[tI[1]])
            sc.op("dve", lambda e: e.tensor_copy(out=tB[0][rp, :], in_=tI[0][rp, :]), reads=[tI[1]], writes=[tB[1]])
            sc.op("dve", lambda e: e.scalar_tensor_tensor(out=tB[0][rp, :], in0=tB[0][rp, :], scalar=-2 * PI, in1=tA[0][rp, :],
                                                          op0=ALU.mult, op1=ALU.add), reads=[tB[1], tA[1]], writes=[tB[1]])
            sc.op("dve", lambda e, which=which: e.tensor_scalar(out=tB[0][rp, :], in0=tB[0][rp, :], scalar1=0.5 * PI * which,
                                                                scalar2=PI, op0=ALU.add, op1=ALU.min),
                  reads=[tB[1]], writes=[tB[1]])
            sc.op("dve", lambda e: e.tensor_scalar(out=tB[0][rp, :], in0=tB[0][rp, :], scalar1=-PI, scalar2=None, op0=ALU.max),
                  reads=[tB[1]], writes=[tB[1]])
            if which == 0:
                sc.op("act", lambda e, tab=tab: e.activation(out=tab[0][rp, :], in_=tB[0][rp, :], func=AF.Sin,
                                                             bias=pic[rp, 0:1], scale=cv[rp, 1:2]),
                      reads=[tB[1], cvb], writes=[tab[1]])
            else:
                sc.op("act", lambda e, tab=tab: e.activation(out=tab[0][rp, :], in_=tB[0][rp, :], func=AF.Sin,
                                                             bias=pic[rp, 0:1], scale=1.0),
                      reads=[tB[1], cvb], writes=[tab[1]])

        def rope_combine(dst, dbuf, pa, ps_):
            sc.op("dve", lambda e: e.tensor_tensor(out=tm1[0][rp, :], in0=pa[0][rp, :], in1=cosT[0][rp, :], op=ALU.mult),
                  reads=[pa[1], cosT[1]], writes=[tm1[1]])
            sc.op("dve", lambda e: e.tensor_tensor(out=tm2[0][rp, :], in0=ps_[0][rp, :], in1=sinT[0][rp, :], op=ALU.mult),
                  reads=[ps_[1], sinT[1]], writes=[tm2[1]])
            sc.op("dve", lambda e: e.tensor_tensor(out=dst[rp, c0:c0 + 512], in0=tm1[0][rp, :], in1=tm2[0][rp, :], op=ALU.add),
                  reads=[tm1[1], tm2[1]], writes=[dbuf])

        for ch in range(3):
            pb = PS[1 + ch]
            for k in range(8):
                sc.op("pe", lambda e, pb=pb, ch=ch, k=k: e.matmul(pb[0][:, :], lhsT=wcq[0][:, k, ch * 128:(ch + 1) * 128],
                                                                 rhs=hg[0][:, k, :], start=(k == 0), stop=(k == 7)),
                      reads=[wcq[1], hg[1]], writes=[pb[1]])
            sc.op("act", lambda e, pb=pb, ch=ch: e.activation(out=cqf[0][:, ch, :], in_=pb[0][:, :], func=AF.Copy),
                  reads=[pb[1]], writes=[cqf[1][ch]])
        for ch in range(2):
            pb = PS[4 + ch]
            for k in range(8):
                sc.op("pe", lambda e, pb=pb, ch=ch, k=k: e.matmul(pb[0][:, :], lhsT=wckv[0][:, k, ch * 128:(ch + 1) * 128],
                                                                 rhs=hg[0][:, k, :], start=(k == 0), stop=(k == 7)),
                      reads=[wckv[1], hg[1]], writes=[pb[1]])
            sc.op("act", lambda e, pb=pb, ch=ch: e.activation(out=ckvf[0][:, ch, :], in_=pb[0][:, :], func=AF.Copy),
                  reads=[pb[1]], writes=[ckvf[1][ch]])
        for (w_, pb) in ((wkr, PS[6]), (wkrs, PS[7])):
            for k in range(8):
                sc.op("pe", lambda e, pb=pb, w_=w_, k=k: e.matmul(pb[0][0:96, :], lhsT=w_[0][:, k, :], rhs=hg[0][:, k, :],
                                                                 start=(k == 0), stop=(k == 7)),
                      reads=[w_[1], hg[1]], writes=[pb[1]])
        rope_combine(K1, kb[g], PS[6], PS[7])
        rmsnorm(sc, R, 3, 512, lambda c: cqf[0][:, c, :], cqf[1], gv[:, 32:35], lambda c: cqn[0][:, c, :], cqn[1], 384)
        for (w_, pb) in ((wuq, PS[1]), (wuqs, PS[2])):
            for ch in range(3):
                sc.op("pe", lambda e, pb=pb, w_=w_, ch=ch: e.matmul(pb[0][0:96, :], lhsT=w_[0][:, ch, :], rhs=cqn[0][:, ch, :],
                                                                   start=(ch == 0), stop=(ch == 2)),
                      reads=[w_[1], cqn[1]], writes=[pb[1]])
        sc.op("act", lambda e: e.activation(out=Q1[0:64, c0:c0 + 512], in_=PS[1][0][0:64, :], func=AF.Copy),
              reads=[PS[1][1]], writes=[qb[g]])
        rope_combine(Q1, qb[g], PS[1], PS[2])
        rmsnorm(sc, R, 2, 512, lambda c: ckvf[0][:, c, :], ckvf[1], gv[:, 35:37], lambda c: ckvn[0][:, c, :], ckvn[1], 256)
        pb = PS[3]
        for ch in range(2):
            sc.op("pe", lambda e, pb=pb, ch=ch: e.matmul(pb[0][0:64, :], lhsT=wuk[0][:, ch, :], rhs=ckvn[0][:, ch, :],
                                                        start=(ch == 0), stop=(ch == 1)),
                  reads=[wuk[1], ckvn[1]], writes=[pb[1]])
        sc.op("act", lambda e, pb=pb: e.activation(out=K1[0:64, c0:c0 + 512], in_=pb[0][0:64, :], func=AF.Copy),
              reads=[pb[1]], writes=[kb[g]])
        pv = PS[4]
        for j in range(4):
            for ch in range(2):
                sc.op("pe", lambda e, pv=pv, j=j, ch=ch: e.matmul(pv[0][:, j * 64:(j + 1) * 64], lhsT=ckvn[0][:, ch, j * 128:(j + 1) * 128],
                                                                 rhs=wuv[0][:, ch, :], start=(ch == 0), stop=(ch == 1)),
                      reads=[wuv[1], ckvn[1]], writes=[pv[1]])
        sc.op("act", lambda e, pv=pv, g=g: e.activation(out=V[:, g * 4:(g + 1) * 4, 0:64],
                                                        in_=pv[0][:, 0:256].rearrange("p (j d) -> p j d", j=4), func=AF.Copy),
              reads=[pv[1]], writes=[vb[g]])
    for g in range(NGR):
        mla_prep(g)
    if STAGE == 1:
        return
    attention_pass(lambda kt: (Q1, 0), K1, 96, 96.0 ** -0.5, 0, "mla")
    if STAGE == 2:
        return
    sc.barrier()
    cx.release(m0)

    wmq = load_w(sc, cx, dr, "w_mq", [128, 8, 128])
    wmk = load_w(sc, cx, dr, "w_mk", [128, 8, 128])
    wmv = load_w(sc, cx, dr, "w_mv", [128, 8, 64])
    kms2 = [(cx.sb(f"kms{i}", [128, 1], F32), Buf(f"kms{i}")) for i in range(2)]
    kmT = (cx.sb("kmT", [128, 64], BF16), Buf("kmT"))
    gm = (cx.sb("gm", [128, 64], F32), Buf("gm"))
    pm = (cx.sb("pm", [128, 64], F32), Buf("pm"))
    m8 = (cx.sb("m8", [128, 8], F32), Buf("m8"))
    selt = (cx.sb("selt", [128, 64], BF16), Buf("selt"))
    lo = (cx.sb("lo", [128, 512], F32), Buf("lo"))
    hib = (cx.sb("hib", [128, 512], BF16), Buf("hib"))
    sc.op("pool", lambda e: e.memset(kmT[0][:, :], 0.0), writes=[kmT[1]])
    for g4 in range(4):
        sc.dma("pool", K1[32:64, g4 * 4096:(g4 + 1) * 4096], dr["onehot"][:, g4 * 4096:(g4 + 1) * 4096],
               writes=[kb[g] for g in range(g4 * 8, g4 * 8 + 8)])
    r0 = slice(0, 32)
    qr = slice(64, 128)
    def moba_prep(g):
        hg = load_h2(g)
        load_pos(g)
        c0 = g * 512
        sc.op("dve", lambda e: e.tensor_copy(out=hib[0][r0, :], in_=posf[0][r0, :]), reads=[posf[1]], writes=[hib[1]])
        sc.op("dve", lambda e: e.tensor_copy(out=tA[0][r0, :], in_=hib[0][r0, :]), reads=[hib[1]], writes=[tA[1]])
        sc.op("dve", lambda e: e.tensor_tensor(out=lo[0][r0, :], in0=posf[0][r0, :], in1=tA[0][r0, :], op=ALU.subtract),
              reads=[posf[1], tA[1]], writes=[lo[1]])
        dsts = [(Q1, qb[g], 2, c0), (K1, kb[g], 5, c0)]
        if g >= NG // 2:
            dsts.append((QB, qbb[g], 2, c0 - S // 2))
        for (dst, dbuf, cc, co) in dsts:
            sc.op("dve", lambda e, cc=cc: e.tensor_scalar(out=tB[0][r0, :], in0=tA[0][r0, :], scalar1=cv[r0, cc:cc + 1],
                                                          scalar2=cv[r0, cc + 2:cc + 3], op0=ALU.mult, op1=ALU.add),
                  reads=[tA[1], cvb], writes=[tB[1]])
            sc.op("dve", lambda e, cc=cc, dst=dst, co=co: e.scalar_tensor_tensor(
                out=dst[r0, co:co + 512], in0=lo[0][r0, :], scalar=cv[r0, cc + 1:cc + 2], in1=tB[0][r0, :],
                op0=ALU.mult, op1=ALU.add), reads=[lo[1], tB[1], cvb], writes=[dbuf])
        if MSUB == 1:
            return
        pq, pk = PS[1], PS[2]
        for (w_, pb) in ((wmq, pq), (wmk, pk)):
            for k in range(8):
                sc.op("pe", lambda e, pb=pb, w_=w_, k=k: e.matmul(pb[0][:, :], lhsT=w_[0][:, k, :], rhs=hg[0][:, k, :],
                                                                 start=(k == 0), stop=(k == 7)),
                      reads=[w_[1], hg[1]], writes=[pb[1]])
        sc.op("act", lambda e: e.activation(out=Q1[qr, c0:c0 + 512], in_=pq[0][qr, :], func=AF.Copy, scale=0.125),
              reads=[pq[1]], writes=[qb[g]])
        if g >= NG // 2:
            sc.op("act", lambda e: e.activation(out=QB[qr, c0 - S // 2:c0 - S // 2 + 512], in_=pq[0][qr, :], func=AF.Copy, scale=0.125),
                  reads=[pq[1]], writes=[qbb[g]])
        sc.op("act", lambda e: e.activation(out=K1[qr, c0:c0 + 512], in_=pk[0][qr, :], func=AF.Copy),
              reads=[pk[1]], writes=[kb[g]])
        if MSUB == 2:
            return
        pv = PS[4]
        for j in range(4):
            for k in range(8):
                sc.op("pe", lambda e, j=j, k=k: e.matmul(pv[0][:, j * 64:(j + 1) * 64], lhsT=hg[0][:, k, j * 128:(j + 1) * 128],
                                                        rhs=wmv[0][:, k, :], start=(k == 0), stop=(k == 7)),
                      reads=[wmv[1], hg[1]], writes=[pv[1]])
        sc.op("act", lambda e, g=g: e.activation(out=V[:, g * 4:(g + 1) * 4, 0:64],
                                                 in_=pv[0][:, 0:256].rearrange("p (j d) -> p j d", j=4), func=AF.Copy),
              reads=[pv[1]], writes=[vb[g]])
        if MSUB == 3:
            return
        sc.op("act", lambda e: e.activation(out=tm1[0][qr, :], in_=pk[0][qr, :], func=AF.Copy), reads=[pk[1]], writes=[tm1[1]])
        for b2 in range(2):
            bi = g * 2 + b2
            ks = kms2[b2]
            sc.op("dve", lambda e, b2=b2, ks=ks: e.reduce_sum(out=ks[0][qr, :], in_=tm1[0][qr, b2 * 256:(b2 + 1) * 256], axis=AX.X),
                  reads=[tm1[1]], writes=[ks[1]])
            sc.op("act", lambda e, bi=bi, ks=ks: e.activation(out=kmT[0][qr, bi:bi + 1], in_=ks[0][qr, 0:1], func=AF.Copy, scale=1.0 / 256),
                  reads=[ks[1]], writes=[kmT[1]])
        if MSUB == 4:
            return
        for t in range(4):
            tile_i = g * 4 + t
            own = tile_i // 2
            pg = PS[5]
            sc.op("pe", lambda e, t=t: e.matmul(pg[0][:, 0:64], lhsT=Q1[qr, c0 + t * 128:c0 + (t + 1) * 128], rhs=kmT[0][qr, :],
                                                start=True, stop=True), reads=[qb[g], kmT[1]], writes=[pg[1]])
            sc.op("dve", lambda e, own=own: e.tensor_scalar(out=pm[0][:, :], in0=blkidx[:, :], scalar1=float(own), scalar2=-1e30,
                                                            op0=ALU.is_ge, op1=ALU.mult), reads=[blkb], writes=[pm[1]])
            sc.op("dve", lambda e: e.tensor_tensor(out=gm[0][:, :], in0=pg[0][:, 0:64], in1=pm[0][:, :], op=ALU.add),
                  reads=[pg[1], pm[1]], writes=[gm[1]])
            sc.op("dve", lambda e: e.max(out=m8[0][:, :], in_=gm[0][:, :]), reads=[gm[1]], writes=[m8[1]])
            sc.op("dve", lambda e: e.tensor_scalar(out=m8[0][:, 2:3], in0=m8[0][:, 2:3], scalar1=-1e29, scalar2=None, op0=ALU.max),
                  reads=[m8[1]], writes=[m8[1]])
            sc.op("dve", lambda e: e.tensor_scalar(out=selt[0][:, :], in0=gm[0][:, :], scalar1=m8[0][:, 2:3], scalar2=1.0,
                                                   op0=ALU.is_ge, op1=ALU.subtract), reads=[gm[1], m8[1]], writes=[selt[1]])
            sc.op("dve", lambda e, own=own: e.memset(selt[0][:, own:own + 1], 0.0), reads=[], writes=[selt[1]])
            tp = PS[6]
            tpv = tp[0].bitcast(BF16)
            sc.op("pe", lambda e, tpv=tpv: e.transpose(out=tpv[0:64, 0:128], in_=selt[0][:, :], identity=ident[0][:, :]),
                  reads=[selt[1], ident[1]], writes=[tp[1]])
            cs = c0 + t * 128
            sc.op("act", lambda e, tpv=tpv, cs=cs: e.activation(out=Q1[32:64, cs:cs + 128], in_=tpv[0:32, 0:128], func=AF.Copy),
                  reads=[tp[1]], writes=[qb[g]])
            if g >= NG // 2:
                sc.op("act", lambda e, tpv=tpv, cs=cs: e.activation(out=QB[32:64, cs - S // 2:cs - S // 2 + 128], in_=tpv[32:64, 0:128],
                                                                    func=AF.Copy), reads=[tp[1]], writes=[qbb[g]])
    for g in range(NGR):
        if MSUB > 0:
            moba_prep(g)
    if STAGE == 3:
        return
    attention_pass(lambda kt: (Q1, 0) if kt < 64 else (QB, S // 2), K1, 128, 1.0, 64, "moba")


ARENA_ELEMS = 96 * 1024

IN_SPECS = [
    ("xT", [D, TC]), ("f1_wg", [NF, 128, 8, 128]), ("f1_wu", [NF, 128, 8, 128]), ("f1_wd", [8, 128, NF, 128]),
    ("f2_wg", [NF, 128, 8, 128]), ("f2_wu", [NF, 128, 8, 128]), ("f2_wd", [8, 128, NF, 128]),
    ("gvec", [128, 37]), ("cvec", [128, 8]),
    ("w_cq", [128, 8, 384]), ("w_ckv", [128, 8, 256]), ("w_kr", [128, 8, 96]), ("w_krs", [128, 8, 96]),
    ("w_mq", [128, 8, 128]), ("w_mk", [128, 8, 128]), ("w_mv", [128, 8, 64]),
    ("w_uq", [128, 3, 96]), ("w_uqs", [128, 3, 96]), ("w_uk", [128, 2, 64]), ("w_uv", [128, 2, 64]),
    ("w_out", [128, 8, D]), ("onehot", [32, S]), ("ident", [128, 128]), ("maskc", [128, 4, 512]),
    ("blkidx", [128, 64]),
]


def build(mode="full"):
    nc = bass.Bass("TRN2", target_bir_lowering=False)
    dr = {}
    for name, shp in IN_SPECS:
        dr[name] = nc.dram_tensor(name, shp, F32, kind="ExternalInput").ap()
    dr["pos"] = nc.dram_tensor("pos", [S], I32, kind="ExternalInput").ap()
    if mode == "attn":
        h2_all = nc.dram_tensor("h2_all", [NCORES * D, TC], F32, kind="ExternalInput").ap()
        cat_loc = nc.dram_tensor("cat_loc", [128, S], BF16, kind="ExternalOutput").ap()
    else:
        h2_all = nc.dram_tensor("h2_all", [NCORES * D, TC], BF16).ap()
        cat_loc = nc.dram_tensor("cat_loc", [128, S], BF16).ap()
        h2_loc = nc.dram_tensor("h2_loc", [D, TC], BF16).ap()
        cat_all = nc.dram_tensor("cat_all", [NCORES * 128, S], BF16).ap()
        x1_d = nc.dram_tensor("x1_d", [D, TC], F32).ap()
        out_d = nc.dram_tensor("outT", [D, TC], F32, kind="ExternalOutput").ap()
    with contextlib.ExitStack() as stack:
        sc = Sched(nc, stack)
        cx = Ctx(nc, stack, ARENA_ELEMS)
        R = alloc_common(sc, cx, dr)
        gv = R["gv"]
        mA = cx.mark()
        catb = Buf("cat_loc")
        if mode == "attn":
            R["h2all_b"] = Buf("h2_all")
            emit_attention(sc, cx, R, dr, h2_all, cat_loc, catb)
            sc.barrier()
            sc.final_wait("sp", [catb])
        else:
            cc1 = stack.enter_context(nc.semaphore("cc1"))
            cc2 = stack.enter_context(nc.semaphore("cc2"))
            NTG = TC // 512
            xT = cx.sb("xT", [128, 8, TC], F32)
            xb = [[Buf(f"x{c}_{tg}") for tg in range(NTG)] for c in range(8)]
            xv = dr["xT"].rearrange("(c p) t -> p c t", p=128)
            for tg in range(NTG):
                sc.dma("sp", xT[:, :, tg * 512:(tg + 1) * 512], xv[:, :, tg * 512:(tg + 1) * 512], writes=[xb[c][tg] for c in range(8)])
            F = alloc_ffn(sc, cx)
            emit_ffn(sc, R, F, xT, xb, dr["f1_wg"], dr["f1_wu"], dr["f1_wd"], gv[:, 0:8])
            hT, hb = F["hT"]
            h2lb = Buf("h2_loc")
            h2lv = h2_loc.rearrange("(c p) t -> p c t", p=128)
            for tg in range(NTG):
                j = tg % 2
                rmsnorm(sc, R, 8, 512, lambda c, tg=tg: xT[:, c, tg * 512:(tg + 1) * 512], [xb[c][tg] for c in range(8)],
                        gv[:, 8:16], lambda c, j=j: hT[:, c, j * 512:(j + 1) * 512], hb[j], D)
                sc.dma("sp", h2lv[:, :, tg * 512:(tg + 1) * 512], hT[:, :, j * 512:(j + 1) * 512], reads=[hb[j]], writes=[h2lb], epoch="h2",
                       carrier=hb[j])
            x1b = Buf("x1_d")
            x1v = x1_d.rearrange("(c p) t -> p c t", p=128)
            for tg in range(NTG):
                sc.dma("sp", x1v[:, :, tg * 512:(tg + 1) * 512], xT[:, :, tg * 512:(tg + 1) * 512], reads=[xb[c][tg] for c in range(8)],
                       writes=[x1b], epoch="x1", carrier=Buf(f"x1s{tg}"))
            R["h2all_b"] = Buf("h2_all")
            sc.raw("pool", lambda e: e.collective_compute("AllGather", ALU.bypass, replica_groups=[list(range(NCORES))],
                                                          ins=[h2_loc.opt()], outs=[h2_all.opt()]),
                   cc1, 1, reads=[h2lb], writes=[R["h2all_b"]])
            sc.barrier()
            cx.release(mA)
            emit_attention(sc, cx, R, dr, h2_all, cat_loc, catb)
            catab = Buf("cat_all")
            sc.raw("pool", lambda e: e.collective_compute("AllGather", ALU.bypass, replica_groups=[list(range(NCORES))],
                                                          ins=[cat_loc.opt()], outs=[cat_all.opt()]),
                   cc2, 1, reads=[catb], writes=[catab])
            sc.barrier()
            cx.release(mA)
            xT = cx.sb("xT", [128, 8, TC], F32)
            xb = [[Buf(f"y{c}_{tg}") for tg in range(NTG)] for c in range(8)]
            for tg in range(NTG):
                sc.dma("sp", xT[:, :, tg * 512:(tg + 1) * 512], x1v[:, :, tg * 512:(tg + 1) * 512], reads=[x1b],
                       writes=[xb[c][tg] for c in range(8)])
            mC = cx.mark()
            wout = load_w(sc, cx, dr, "w_out", [128, 8, D])
            catg = [(cx.sb(f"catg{i}", [128, 8, 512], BF16), Buf(f"catg{i}")) for i in range(2)]
            cav = cat_all.rearrange("(r p) t -> p r t", p=128)
            for tg in range(NTG):
                cg = catg[tg % 2]
                sc.dma_fn("sp", lambda e, cg=cg, tg=tg: e.dma_start(
                    out=cg[0][:, :, :], in_=cav[:, :, bass.ds(e.partition_id() * TC + tg * 512, 512)]),
                          reads=[catab], writes=[cg[1]])
                for d in range(8):
                    pb = cx.PS[5 + (d % 2)]
                    for r in range(8):
                        sc.op("pe", lambda e, pb=pb, r=r, d=d, cg=cg: e.matmul(
                            pb[0][:, :], lhsT=wout[0][:, r, d * 128:(d + 1) * 128], rhs=cg[0][:, r, :],
                            start=(r == 0), stop=(r == 7)), reads=[wout[1], cg[1]], writes=[pb[1]])
                    sc.op("dve", lambda e, pb=pb, d=d, tg=tg: e.tensor_tensor(
                        out=xT[:, d, tg * 512:(tg + 1) * 512], in0=pb[0][:, :], in1=xT[:, d, tg * 512:(tg + 1) * 512], op=ALU.add),
                          reads=[pb[1], xb[d][tg]], writes=[xb[d][tg]])
            sc.barrier()
            cx.release(mC)
            F = alloc_ffn(sc, cx)
            ostage = (cx.sb("ostage", [128, 8, 512], F32), Buf("ostage"))
            emit_ffn(sc, R, F, xT, xb, dr["f2_wg"], dr["f2_wu"], dr["f2_wd"], gv[:, 16:24])
            ov = out_d.rearrange("(c p) t -> p c t", p=128)
            ob = Buf("out")
            for tg in range(NTG):
                rmsnorm(sc, R, 8, 512, lambda c, tg=tg: xT[:, c, tg * 512:(tg + 1) * 512], [xb[c][tg] for c in range(8)],
                        gv[:, 24:32], lambda c: ostage[0][:, c, :], ostage[1], D)
                sc.dma("sp", ov[:, :, tg * 512:(tg + 1) * 512], ostage[0][:, :, :], reads=[ostage[1]], writes=[ob], epoch="out",
                       carrier=ostage[1])
            sc.final_wait("sp", [ob])
        block = stack.enter_context(nc.Block())
        sc.emit(block)
    return nc


def _pcn(w):
    K, N = w.shape
    return np.ascontiguousarray(w.reshape(K // 128, 128, N).transpose(1, 0, 2))


def _lay_gu(w):
    return np.ascontiguousarray(w.reshape(8, 128, NF, 128).transpose(2, 1, 0, 3))


def _lay_d(w):
    return np.ascontiguousarray(w.reshape(NF, 128, 8, 128).transpose(2, 1, 0, 3))


def host_inputs(inp):
    f32 = np.float32
    w_in = np.asarray(inp["w_in"][0], f32)
    w_uq = np.asarray(inp["mla_w_uq"][0], f32)
    w_ukv = np.asarray(inp["mla_w_ukv"][0], f32)
    w_out = np.asarray(inp["w_out"][0], f32)
    x = np.asarray(inp["x"][0], f32)
    common = {
        "f1_wg": _lay_gu(np.asarray(inp["ffn1_w_gate"][0], f32)), "f1_wu": _lay_gu(np.asarray(inp["ffn1_w_up"][0], f32)),
        "f1_wd": _lay_d(np.asarray(inp["ffn1_w_down"][0], f32)),
        "f2_wg": _lay_gu(np.asarray(inp["ffn2_w_gate"][0], f32)), "f2_wu": _lay_gu(np.asarray(inp["ffn2_w_up"][0], f32)),
        "f2_wd": _lay_d(np.asarray(inp["ffn2_w_down"][0], f32)),
        "pos": np.ascontiguousarray(np.asarray(inp["positions"][0], np.int32)),
    }
    gvec = np.zeros((128, 37), f32)
    for i, nm in enumerate(["ffn1_norm", "mix_norm", "ffn2_norm"]):
        gvec[:, i * 8:(i + 1) * 8] = np.asarray(inp[nm][0], f32).reshape(8, 128).T
    gvec[:, 24:32] = np.asarray(inp["final_norm"], f32).reshape(8, 128).T
    gvec[:, 32:35] = np.asarray(inp["mla_q_norm"][0], f32).reshape(3, 128).T
    gvec[:, 35:37] = np.asarray(inp["mla_kv_norm"][0], f32).reshape(2, 128).T
    common["gvec"] = gvec
    common["w_cq"] = _pcn(w_in[:, 0:384])
    common["w_ckv"] = _pcn(w_in[:, 384:640])
    kr = np.zeros((D, 96), f32)
    kr[:, 64:96] = w_in[:, 640:672]
    krs = np.zeros((D, 96), f32)
    krs[:, 64:80] = w_in[:, 656:672]
    krs[:, 80:96] = w_in[:, 640:656]
    common["w_kr"] = _pcn(kr)
    common["w_krs"] = _pcn(krs)
    j = np.arange(S)
    oh = np.zeros((32, S), f32)
    oh[(j // 256) % 32, j] = BIG
    common["onehot"] = oh
    common["ident"] = np.eye(128, dtype=f32)
    mk = np.zeros((128, 4, 512), f32)
    kk = np.arange(128)[:, None]
    qq = np.arange(512)[None, :]
    for i in range(4):
        mk[:, i, :] = np.where(i * 128 + kk > qq, -BIG, 0.0)
    common["maskc"] = mk
    common["blkidx"] = np.tile(np.arange(64, dtype=f32)[None, :], (128, 1))
    wo = np.zeros((NCORES, 128, D), f32)
    for r in range(NCORES):
        wo[r, 0:64] = w_out[r * 64:(r + 1) * 64]
        wo[r, 64:128] = w_out[512 + r * 64:512 + (r + 1) * 64]
    common["w_out"] = np.ascontiguousarray(wo.transpose(1, 0, 2))
    inv = np.power(np.float32(10000.0), -np.arange(16, dtype=f32) * 2.0 / 32.0).astype(f32)
    maps = []
    for c in range(NCORES):
        m = dict(common)
        m["xT"] = np.ascontiguousarray(x[c * TC:(c + 1) * TC].T)
        cvec = np.zeros((128, 8), f32)
        cvec[64:96, 0] = np.tile(inv, 2)
        cvec[64:80, 1] = -1.0
        cvec[80:96, 1] = 1.0
        slope = 2.0 ** (-8.0 * (c + 1) / 8.0)
        cvec[0, 2] = -slope
        cvec[1, 3] = -slope
        cvec[2, 4] = 1.0
        cvec[3, 4] = 1.0
        cvec[2, 5] = slope
        cvec[3, 6] = slope
        cvec[0, 7] = 1.0
        cvec[1, 7] = 1.0
        m["cvec"] = cvec
        mq = np.zeros((D, 128), f32)
        mq[:, 64:128] = w_in[:, 672 + c * 64:672 + (c + 1) * 64]
        mkk = np.zeros((D, 128), f32)
        mkk[:, 64:128] = w_in[:, 1184 + c * 64:1184 + (c + 1) * 64]
        m["w_mq"] = _pcn(mq)
        m["w_mk"] = _pcn(mkk)
        m["w_mv"] = _pcn(w_in[:, 1696 + c * 64:1696 + (c + 1) * 64])
        uq = w_uq[:, c * 96:(c + 1) * 96]
        uqs = np.zeros((384, 96), f32)
        uqs[:, 64:80] = uq[:, 80:96]
        uqs[:, 80:96] = uq[:, 64:80]
        m["w_uq"] = _pcn(uq)
        m["w_uqs"] = _pcn(uqs)
        m["w_uk"] = _pcn(w_ukv[:, c * 128:c * 128 + 64])
        m["w_uv"] = _pcn(w_ukv[:, c * 128 + 64:c * 128 + 128])
        maps.append(m)
    return maps


def kernel(**inputs):
    maps = host_inputs(inputs)
    nc = build("full")
    res = run_bass_kernel_spmd(nc, maps, core_ids=list(range(NCORES)))
    out = np.empty((1, S, D), np.float32)
    for c in range(NCORES):
        out[0, c * TC:(c + 1) * TC, :] = np.asarray(res.results[c]["outT"], np.float32).T
    return out
```
